# Optimizing a Trainium2 kernel written in Bass

```python
import math
import jax, jax.numpy as jnp
from jax import lax
import numpy as np

D_MODEL = 1024
BATCH = 2
SEQ = 8192
DEPTH = 4

NORM_EPS = 1e-6
CONV_A_WIDTH = D_MODEL
CONV_A_K = 3
DN_HEAD_DIM = 128
DN_HEADS = D_MODEL // DN_HEAD_DIM
DN_WIDTH = DN_HEADS * DN_HEAD_DIM
DN_CONV_K = 4
DN_CHUNK = 64
MOBA_HEAD_DIM = 128
MOBA_HEADS = D_MODEL // MOBA_HEAD_DIM
MOBA_WIDTH = MOBA_HEADS * MOBA_HEAD_DIM
MOBA_BLOCK = 256
MOBA_TOPK = 3
MOBA_Q_CHUNK = 32
ROPE_THETA = 10000.0
N_BRANCHES = 3
FFN_HIDDEN = -(-(8 * D_MODEL) // (3 * 256)) * 256
IN_SPLITS = (CONV_A_WIDTH, CONV_A_WIDTH, CONV_A_WIDTH,
             3 * DN_WIDTH, DN_WIDTH, DN_HEADS, DN_HEADS,
             3 * MOBA_WIDTH, N_BRANCHES * D_MODEL)
N_IN = sum(IN_SPLITS)

kernel_name = "hybrid_conv_deltanet_moba_block"


def rms_norm(x, w):
    xf = x.astype(jnp.float32)
    y = xf * lax.rsqrt(jnp.mean(xf * xf, axis=-1, keepdims=True) + NORM_EPS)
    return (y * w.astype(jnp.float32)).astype(x.dtype)


def l2_norm(x):
    return x * lax.rsqrt(jnp.sum(x * x, axis=-1, keepdims=True) + NORM_EPS)


def split_columns(t, sizes):
    offsets = np.cumsum(np.array(sizes))[:-1].tolist()
    return jnp.split(t, offsets, axis=-1)


def causal_dwconv(x, w):
    K = w.shape[0]
    S = x.shape[1]
    xp = jnp.pad(x, ((0, 0), (K - 1, 0), (0, 0)))
    return sum(xp[:, j:j + S] * w[j] for j in range(K))


def rope_tables(seq, dim):
    inv = 1.0 / (ROPE_THETA ** (jnp.arange(0, dim, 2, dtype=jnp.float32) / dim))
    ang = jnp.arange(seq, dtype=jnp.float32)[:, None] * inv[None, :]
    return jnp.cos(ang), jnp.sin(ang)


def apply_rope(x, cos, sin):
    xf = x.astype(jnp.float32)
    x1, x2 = jnp.split(xf, 2, axis=-1)
    out = jnp.concatenate([x1 * cos - x2 * sin, x2 * cos + x1 * sin], axis=-1)
    return out.astype(x.dtype)


def chunk_gated_delta_rule(q, k, v, g, beta):
    B, H, S, Dk = q.shape
    Dv = v.shape[-1]
    C = DN_CHUNK
    N = S // C
    q = (q * (Dk ** -0.5)).reshape(B, H, N, C, Dk)
    k = k.reshape(B, H, N, C, Dk)
    v = v.reshape(B, H, N, C, Dv)
    g = jnp.cumsum(g.reshape(B, H, N, C), axis=-1)
    beta = beta.reshape(B, H, N, C)
    causal = jnp.tril(jnp.ones((C, C), dtype=bool))
    strict = jnp.tril(jnp.ones((C, C), dtype=bool), k=-1)
    diff = g[..., :, None] - g[..., None, :]
    decay = jnp.where(causal, jnp.exp(jnp.where(causal, diff, 0.0)), 0.0)
    k_beta = k * beta[..., None]
    a_mat = jnp.where(strict, jnp.einsum('bhnid,bhnjd->bhnij', k_beta, k) * decay, 0.0)
    eye = jnp.eye(C, dtype=jnp.float32)
    t_mat = lax.linalg.triangular_solve(eye + a_mat, jnp.broadcast_to(eye, a_mat.shape),
                                        left_side=True, lower=True)
    u = jnp.einsum('bhnij,bhnjd->bhnid', t_mat, v * beta[..., None])
    w = jnp.einsum('bhnij,bhnjd->bhnid', t_mat, k_beta * jnp.exp(g)[..., None])
    qk = jnp.einsum('bhnid,bhnjd->bhnij', q, k) * decay
    q_dec = q * jnp.exp(g)[..., None]
    k_dec = k * jnp.exp(g[..., -1:] - g)[..., None]
    chunk_decay = jnp.exp(g[..., -1])

    def step(state, xs):
        u_n, w_n, qk_n, qd_n, kd_n, cd_n = xs
        v_new = u_n - jnp.einsum('bhcd,bhde->bhce', w_n, state)
        o_n = (jnp.einsum('bhcd,bhde->bhce', qd_n, state)
               + jnp.einsum('bhij,bhje->bhie', qk_n, v_new))
        state = state * cd_n[..., None, None] + jnp.einsum('bhcd,bhce->bhde', kd_n, v_new)
        return state, o_n

    xs = tuple(jnp.moveaxis(t, 2, 0) for t in (u, w, qk, q_dec, k_dec, chunk_decay))
    state0 = jnp.zeros((B, H, Dk, Dv), jnp.float32)
    _, o = lax.scan(step, state0, xs)
    return jnp.moveaxis(o, 0, 2).reshape(B, H, S, Dv)


def moba_attention(q, k, v):
    B, H, S, Dh = q.shape
    BS = MOBA_BLOCK
    QC = MOBA_Q_CHUNK
    NB = -(-S // BS)
    S_pad = NB * BS
    pad = ((0, 0), (0, 0), (0, S_pad - S), (0, 0))
    q, k, v = (jnp.pad(t, pad) for t in (q, k, v))
    scale = Dh ** -0.5
    topk = min(MOBA_TOPK, NB)
    kb = k.reshape(B, H, NB, BS, Dh)
    vb = v.reshape(B, H, NB, BS, Dh)
    k_mean = jnp.mean(kb.astype(jnp.float32), axis=3)
    pos = jnp.arange(S_pad)
    q_blk = pos // BS
    gate = jnp.einsum('bhsd,bhnd->bhsn', q.astype(jnp.float32), k_mean)
    fully_past = jnp.arange(NB)[None, :] < q_blk[:, None]
    gate = jnp.where(fully_past, gate, -jnp.inf)
    _, sel = lax.top_k(gate, topk)
    sel_valid = jnp.arange(topk)[None, :] < q_blk[:, None]

    nq = S_pad // QC
    q_c = jnp.moveaxis(q.reshape(B, H, nq, QC, Dh), 2, 0)
    sel_c = jnp.moveaxis(sel.reshape(B, H, nq, QC, topk), 2, 0)
    valid_c = sel_valid.reshape(nq, QC, topk)
    pos_c = pos.reshape(nq, QC)
    gather_blocks = jax.vmap(jax.vmap(lambda blocks, idx: blocks[idx]))

    def query_chunk(args):
        qc, selc, validc, posc = args
        own = posc[0] // BS
        k_own = lax.dynamic_index_in_dim(kb, own, axis=2, keepdims=False)
        v_own = lax.dynamic_index_in_dim(vb, own, axis=2, keepdims=False)
        k_sel = gather_blocks(kb, selc)
        v_sel = gather_blocks(vb, selc)
        s_sel = jnp.einsum('bhqd,bhqnkd->bhqnk', qc, k_sel).astype(jnp.float32) * scale
        s_sel = jnp.where(validc[None, None, :, :, None], s_sel, -jnp.inf)
        s_own = jnp.einsum('bhqd,bhkd->bhqk', qc, k_own).astype(jnp.float32) * scale
        k_pos = own * BS + jnp.arange(BS)
        s_own = jnp.where(k_pos[None, :] <= posc[:, None], s_own, -jnp.inf)
        logits = jnp.concatenate([s_sel.reshape(B, H, QC, topk * BS), s_own], axis=-1)
        p = jax.nn.softmax(logits, axis=-1).astype(v.dtype)
        p_sel = p[..., :topk * BS].reshape(B, H, QC, topk, BS)
        p_own = p[..., topk * BS:]
        return (jnp.einsum('bhqnk,bhqnkd->bhqd', p_sel, v_sel)
                + jnp.einsum('bhqk,bhkd->bhqd', p_own, v_own))

    o = lax.map(query_chunk, (q_c, sel_c, valid_c, pos_c))
    o = jnp.moveaxis(o, 0, 2).reshape(B, H, S_pad, Dh)
    return o[:, :, :S, :]


def hybrid_layer(x, cos, sin, attn_norm, w_in, conv_a_w, dn_conv_w, dn_a_log, dn_dt_bias,
                 dn_norm, w_br_a, w_br_dn, w_br_moba, w_out, ffn_norm, w_gate_up, w_down):
    B, S, _ = x.shape
    h = rms_norm(x, attn_norm)
    proj = h @ w_in
    (a_x, a_c, a_b, dn_qkv, dn_z, dn_b, dn_a, mb_qkv, gate_logits) = split_columns(proj, IN_SPLITS)

    y_a = a_b * causal_dwconv(a_c * a_x, conv_a_w)

    dn_qkv = jax.nn.silu(causal_dwconv(dn_qkv, dn_conv_w)).astype(jnp.float32)
    to_heads = lambda t: t.reshape(B, S, DN_HEADS, DN_HEAD_DIM).transpose(0, 2, 1, 3)
    dq, dk, dv = (to_heads(t) for t in jnp.split(dn_qkv, 3, axis=-1))
    beta = jax.nn.sigmoid(dn_b.astype(jnp.float32)).transpose(0, 2, 1)
    log_decay = (-jnp.exp(dn_a_log.astype(jnp.float32))
                 * jax.nn.softplus(dn_a.astype(jnp.float32) + dn_dt_bias.astype(jnp.float32)))
    o_dn = chunk_gated_delta_rule(l2_norm(dq), l2_norm(dk), dv, log_decay.transpose(0, 2, 1), beta)
    o_dn = o_dn.transpose(0, 2, 1, 3).astype(x.dtype)
    z = dn_z.reshape(B, S, DN_HEADS, DN_HEAD_DIM)
    y_dn = (rms_norm(o_dn, dn_norm) * jax.nn.silu(z)).reshape(B, S, DN_WIDTH)

    to_mheads = lambda t: t.reshape(B, S, MOBA_HEADS, MOBA_HEAD_DIM).transpose(0, 2, 1, 3)
    mq, mk, mv = (to_mheads(t) for t in jnp.split(mb_qkv, 3, axis=-1))
    o_mb = moba_attention(apply_rope(mq, cos, sin), apply_rope(mk, cos, sin), mv)
    y_mb = o_mb.transpose(0, 2, 1, 3).reshape(B, S, MOBA_WIDTH)

    g_a, g_dn, g_mb = jnp.split(jax.nn.sigmoid(gate_logits), N_BRANCHES, axis=-1)
    merged = g_a * (y_a @ w_br_a) + g_dn * (y_dn @ w_br_dn) + g_mb * (y_mb @ w_br_moba)
    x = x + merged @ w_out

    h2 = rms_norm(x, ffn_norm)
    gg, uu = jnp.split(h2 @ w_gate_up, 2, axis=-1)
    return x + (jax.nn.silu(gg) * uu) @ w_down


def setup_inputs(seed: int = 0) -> dict:
    key = jax.random.key(seed)
    ks = jax.random.split(key, 17)
    f32 = jnp.float32
    nrm = lambda k, shape, fan_in: jax.random.normal(k, shape, f32) * (fan_in ** -0.5)
    gain = lambda k, shape: 1.0 + 0.02 * jax.random.normal(k, shape, f32)
    dt = jnp.exp(jax.random.uniform(ks[6], (DEPTH, DN_HEADS), f32, math.log(1e-3), math.log(1e-1)))
    return {
        "x": jax.random.normal(ks[0], (BATCH, SEQ, D_MODEL), f32),
        "attn_norm": gain(ks[1], (DEPTH, D_MODEL)),
        "w_in": nrm(ks[2], (DEPTH, D_MODEL, N_IN), D_MODEL),
        "conv_a_w": nrm(ks[3], (DEPTH, CONV_A_K, CONV_A_WIDTH), CONV_A_K),
        "dn_conv_w": nrm(ks[4], (DEPTH, DN_CONV_K, 3 * DN_WIDTH), DN_CONV_K),
        "dn_a_log": jnp.log(jax.random.uniform(ks[5], (DEPTH, DN_HEADS), f32, 1.0, 16.0)),
        "dn_dt_bias": dt + jnp.log(-jnp.expm1(-dt)),
        "dn_norm": gain(ks[7], (DEPTH, DN_HEAD_DIM)),
        "w_br_a": nrm(ks[8], (DEPTH, CONV_A_WIDTH, D_MODEL), CONV_A_WIDTH),
        "w_br_dn": nrm(ks[9], (DEPTH, DN_WIDTH, D_MODEL), DN_WIDTH),
        "w_br_moba": nrm(ks[10], (DEPTH, MOBA_WIDTH, D_MODEL), MOBA_WIDTH),
        "w_out": nrm(ks[11], (DEPTH, D_MODEL, D_MODEL), D_MODEL),
        "ffn_norm": gain(ks[12], (DEPTH, D_MODEL)),
        "w_gate_up": nrm(ks[13], (DEPTH, D_MODEL, 2 * FFN_HIDDEN), D_MODEL),
        "w_down": nrm(ks[14], (DEPTH, FFN_HIDDEN, D_MODEL), FFN_HIDDEN),
        "final_norm": gain(ks[15], (D_MODEL,)),
    }


def reference(x, attn_norm, w_in, conv_a_w, dn_conv_w, dn_a_log, dn_dt_bias, dn_norm,
              w_br_a, w_br_dn, w_br_moba, w_out, ffn_norm, w_gate_up, w_down, final_norm):
    cos, sin = rope_tables(x.shape[1], MOBA_HEAD_DIM)
    for l in range(DEPTH):
        x = hybrid_layer(x, cos, sin, attn_norm[l], w_in[l], conv_a_w[l], dn_conv_w[l],
                         dn_a_log[l], dn_dt_bias[l], dn_norm[l], w_br_a[l], w_br_dn[l],
                         w_br_moba[l], w_out[l], ffn_norm[l], w_gate_up[l], w_down[l])
    return rms_norm(x, final_norm)
```

```python
import contextlib
import numpy as np
import ml_dtypes
import concourse.bass as bass
import concourse.mybir as mybir
from concourse.bass_utils import run_bass_kernel_spmd

F32 = mybir.dt.float32
BF16 = mybir.dt.bfloat16
AF = mybir.ActivationFunctionType
ALU = mybir.AluOpType
AX = mybir.AxisListType

D = 1024
NCH = 8
DEPTH = 4
FFN = 2816
NHC = 22
EPS = 1e-6
ENGS = ("pe", "act", "dve", "pool", "sp")
NDMASEM = 8

class Prog:
    def __init__(self, nc, stack):
        self.nc = nc
        self.stack = stack
        self.q = {e: [] for e in ENGS}
        self.cnt = {e: 0 for e in ENGS}
        self.sem = {}
        for e in ("pe", "act", "dve", "pool"):
            self.sem[e] = stack.enter_context(nc.semaphore("c_" + e))
        self.dsem = {}
        self.dcount = {}
        self.dnext = {}
        for e in ("sp", "act", "pool"):
            self.dsem[e] = [stack.enter_context(nc.semaphore("d_%s%d" % (e, i))) for i in range(NDMASEM)]
            self.dcount[e] = [0] * NDMASEM
            self.dnext[e] = 0
        self.waited = {}
        self.res = {}
        self.ninstr = 0

    def sb(self, name, shape, dt=F32):
        return self.stack.enter_context(self.nc.sbuf_tensor("s_" + name, list(shape), dt))

    def ps(self, name, shape, dt=F32):
        return self.stack.enter_context(self.nc.psum_tensor("p_" + name, list(shape), dt))

    def _deps(self, reads, writes):
        deps = {}

        def add(tok):
            if tok is None:
                return
            s, v = tok
            if deps.get(s, 0) < v:
                deps[s] = v

        for r in reads:
            st = self.res.get(r)
            if st:
                add(st["w"])
        for w in writes:
            st = self.res.get(w)
            if st:
                add(st["w"])
                for t in st["r"]:
                    add(t)
        return deps

    def _emit_waits(self, eng, deps, same_engine_ok=False):
        for s, v in deps.items():
            if isinstance(s, str) and s == eng and (eng == "pe" or same_engine_ok):
                continue
            key = (eng, s)
            if self.waited.get(key, 0) >= v:
                continue
            self.waited[key] = v
            semh = self.sem[s] if isinstance(s, str) else self.dsem[s[0]][s[1]]
            self.q[eng].append(lambda e, semh=semh, v=v: e.wait_ge(semh, v))
            self.ninstr += 1

    def _update(self, tok, reads, writes):
        for r in reads:
            st = self.res.setdefault(r, {"w": None, "r": []})
            st["r"].append(tok)
            if len(st["r"]) > 12:
                best = {}
                for s, v in st["r"]:
                    if best.get(s, 0) < v:
                        best[s] = v
                st["r"] = list(best.items())
        for w in writes:
            self.res[w] = {"w": tok, "r": []}

    def op(self, eng, fn, reads=(), writes=()):
        deps = self._deps(reads, writes)
        if eng != "pe":
            raw = {}
            for r in reads:
                st = self.res.get(r)
                if st and st["w"] is not None and st["w"][0] == eng:
                    raw[eng] = max(raw.get(eng, 0), st["w"][1])
            if eng in deps:
                if eng in raw:
                    deps[eng] = raw[eng]
                else:
                    del deps[eng]
        self._emit_waits(eng, deps)
        self.cnt[eng] += 1
        v = self.cnt[eng]
        semh = self.sem[eng]
        self.q[eng].append(lambda e, fn=fn, semh=semh: fn(e).then_inc(semh, 1))
        self.ninstr += 1
        self._update((eng, v), reads, writes)

    def dma(self, qeng, out, in_, reads=(), writes=(), **kw):
        deps = self._deps(reads, writes)
        i = self.dnext[qeng]
        self.dnext[qeng] = (i + 1) % NDMASEM
        sid = (qeng, i)
        if self.dcount[qeng][i] > 0:
            deps[sid] = max(deps.get(sid, 0), self.dcount[qeng][i])
        self._emit_waits(qeng, deps)
        self.dcount[qeng][i] += 16
        v = self.dcount[qeng][i]
        semh = self.dsem[qeng][i]
        self.q[qeng].append(lambda e, out=out, in_=in_, semh=semh, kw=kw: e.dma_start(out=out, in_=in_, **kw).then_inc(semh, 16))
        self.ninstr += 1
        self._update((sid, v), reads, writes)

    def finish(self, final_eng="sp"):
        nc = self.nc
        for qe in self.dsem:
            for i in range(NDMASEM):
                if self.dcount[qe][i] > 0:
                    semh, v = self.dsem[qe][i], self.dcount[qe][i]
                    self.q[final_eng].append(lambda e, semh=semh, v=v: e.wait_ge(semh, v))
        for e2 in ("pe", "act", "dve", "pool"):
            if self.cnt[e2] > 0:
                semh, v = self.sem[e2], self.cnt[e2]
                self.q[final_eng].append(lambda e, semh=semh, v=v: e.wait_ge(semh, v))
        with nc.Block() as block:
            @block.sync
            def _(e):
                for f in self.q["sp"]:
                    f(e)

            @block.tensor
            def _(e):
                for f in self.q["pe"]:
                    f(e)

            @block.scalar
            def _(e):
                for f in self.q["act"]:
                    f(e)

            @block.vector
            def _(e):
                for f in self.q["dve"]:
                    f(e)

            @block.gpsimd
            def _(e):
                for f in self.q["pool"]:
                    f(e)

    def barrier(self):
        for eng in ENGS:
            deps = {}
            for e2 in ("pe", "act", "dve", "pool"):
                if e2 != eng and self.cnt[e2] > 0:
                    deps[e2] = self.cnt[e2]
            for qe in self.dsem:
                for i in range(NDMASEM):
                    if self.dcount[qe][i] > 0:
                        deps[(qe, i)] = self.dcount[qe][i]
            self._emit_waits(eng, deps)


class Rot:
    def __init__(self, P, name, n, shape, dt=F32, psum=False):
        mk = P.ps if psum else P.sb
        self.t = [mk("%s%d" % (name, i), shape, dt) for i in range(n)]
        self.names = ["%s%d" % (name, i) for i in range(n)]
        self.i = 0

    def next(self):
        k = self.i
        self.i = (k + 1) % len(self.t)
        return self.t[k], self.names[k]


def mm(P, ps, psn, lhsT, rhs, reads, start, stop):
    P.op("pe", lambda e: e.matmul(ps, lhsT=lhsT, rhs=rhs, start=start, stop=stop), reads=reads, writes=[psn])


def norm_tile(P, T, x, xn, gam, ones, out, outn, psr, tmp):
    sq, sqn, rs, rsn = tmp
    P.op("pool", lambda e: e.tensor_tensor(out=sq[:], in0=x[:], in1=x[:], op=ALU.mult), reads=[xn], writes=[sqn])
    ps, psn = psr.next()
    for k in range(NCH):
        mm(P, ps[:, :T], psn, ones[:], sq[:, k, :], [sqn, "const"], k == 0, k == NCH - 1)
    P.op("dve", lambda e: e.tensor_scalar(out=rs[:], in0=ps[:, :T], scalar1=1.0 / D, scalar2=EPS, op0=ALU.mult, op1=ALU.add),
         reads=[psn], writes=[rsn])
    P.op("act", lambda e: e.activation(out=rs[:], in_=rs[:], func=AF.Sqrt), reads=[rsn], writes=[rsn])
    P.op("dve", lambda e: e.reciprocal(out=rs[:], in_=rs[:]), reads=[rsn], writes=[rsn])
    for k in range(NCH):
        P.op("dve", lambda e, k=k: e.scalar_tensor_tensor(out=out[:, k, :], in0=x[:, k, :], scalar=gam[:, k:k + 1], in1=rs[:],
                                                        op0=ALU.mult, op1=ALU.mult),
             reads=[xn, rsn, "gam"], writes=[outn])


def wview(w):
    return w.rearrange("(c p) n -> p c n", p=128)


def tview(a, t0, T):
    return a.rearrange("(c p) n -> p c n", p=128)[:, :, t0:t0 + T]


def load_w(P, dst, src, name, nsplit=1):
    n = dst.shape[-1]
    step = (n + nsplit - 1) // nsplit
    for s in range(0, n, step):
        P.dma("pool", dst[:, :, s:min(n, s + step)], src[:, :, s:min(n, s + step)], writes=[name])


def build_p0(NT):
    T = 256
    nc = bass.Bass("TRN2", target_bir_lowering=False)
    xT = nc.dram_tensor("xT", [D, NT], F32, kind="ExternalInput").ap()
    gam_d = nc.dram_tensor("gam", [128, NCH], F32, kind="ExternalInput").ap()
    ones_d = nc.dram_tensor("ones", [128, 128], F32, kind="ExternalInput").ap()
    hT = nc.dram_tensor("hT", [D, NT], BF16, kind="ExternalOutput").ap()
    with contextlib.ExitStack() as st:
        P = Prog(nc, st)
        gam = P.sb("gam", [128, NCH])
        ones = P.sb("ones", [128, 128])
        P.dma("sp", gam[:], gam_d, writes=["gam"])
        P.dma("sp", ones[:], ones_d, writes=["const"])
        xr = Rot(P, "x", 2, [128, NCH, T])
        hr = Rot(P, "h", 2, [128, NCH, T], BF16)
        psr = Rot(P, "ps", 2, [128, 512], F32, psum=True)
        sq = P.sb("sq", [128, NCH, T])
        rs = P.sb("rs", [128, T])
        for t0 in range(0, NT, T):
            x, xn = xr.next()
            h, hn = hr.next()
            P.dma("sp", x[:], tview(xT, t0, T), writes=[xn])
            norm_tile(P, T, x, xn, gam, ones, h, hn, psr, (sq, "sq", rs, "rs"))
            P.dma("sp", tview(hT, t0, T), h[:], reads=[hn])
        P.finish()
    return nc


def build_b(NT, last):
    T = 256
    nc = bass.Bass("TRN2", target_bir_lowering=False)
    di = lambda n, s, dt=F32: nc.dram_tensor(n, list(s), dt, kind="ExternalInput").ap()
    xT = di("xT", [D, NT])
    hT = di("hT", [D, NT], BF16)
    yd = [di("y%d" % i, [D, NT], BF16) for i in range(3)]
    wg_d = di("wg", [D, 3 * D])
    wbr_d = [di("wbr%d" % i, [D, D]) for i in range(3)]
    wo_d = di("wo", [D, D])
    wgu_d = di("wgu", [D, 2 * FFN])
    wd_d = di("wd", [FFN, D])
    gf_d = di("gffn", [128, NCH])
    gn_d = di("gnext", [128, NCH])
    ones_d = di("ones", [128, 128])
    xo = nc.dram_tensor("xo", [D, NT], F32, kind="ExternalOutput").ap()
    if last:
        ho = nc.dram_tensor("of", [D, NT], F32, kind="ExternalOutput").ap()
    else:
        ho = nc.dram_tensor("ho", [D, NT], BF16, kind="ExternalOutput").ap()
    xmid_d = nc.dram_tensor("xmid", [D, NT], F32, kind="Internal").ap()
    h2_d = nc.dram_tensor("h2", [D, NT], BF16, kind="Internal").ap()
    with contextlib.ExitStack() as st0:
        P = Prog(nc, st0)
        gf = P.sb("gf", [128, NCH])
        gn = P.sb("gn", [128, NCH])
        ones = P.sb("ones", [128, 128])
        P.dma("sp", gf[:], gf_d, writes=["gam"])
        P.dma("sp", gn[:], gn_d, writes=["gam"])
        P.dma("sp", ones[:], ones_d, writes=["const"])
        psr = Rot(P, "ps", 8, [128, 512], F32, psum=True)
        with contextlib.ExitStack() as st:
            P.stack = st
            wg = P.sb("wg", [128, NCH, 3 * D], BF16)
            wbr = [P.sb("wbr%d" % i, [128, NCH, D], BF16) for i in range(3)]
            wo = P.sb("wo", [128, NCH, D], BF16)
            load_w(P, wg, wview(wg_d), "wg", 6)
            for i in range(3):
                load_w(P, wbr[i], wview(wbr_d[i]), "wbr%d" % i, 2)
            load_w(P, wo, wview(wo_d), "wo", 2)
            hr = Rot(P, "h", 2, [128, NCH, T], BF16)
            yr = [Rot(P, "y%d_" % i, 2, [128, NCH, T], BF16) for i in range(3)]
            xr = Rot(P, "x", 2, [128, NCH, T])
            mg = P.sb("mg", [128, NCH, T], BF16)
            xm = P.sb("xm", [128, NCH, T])
            sq = P.sb("sq", [128, NCH, T])
            rs = P.sb("rs", [128, T])
            h2 = P.sb("h2t", [128, NCH, T], BF16)
            sgr = Rot(P, "sg", 3, [128, T])
            tr = Rot(P, "tt", 2, [128, T])
            macc = P.sb("macc", [128, T])
            for t0 in range(0, NT, T):
                h, hn = hr.next()
                P.dma("sp", h[:], tview(hT, t0, T), writes=[hn])
                ys = []
                for i in range(3):
                    y, yn = yr[i].next()
                    P.dma("sp", y[:], tview(yd[i], t0, T), writes=[yn])
                    ys.append((y, yn))
                x, xn = xr.next()
                P.dma("sp", x[:], tview(xT, t0, T), writes=[xn])
                for m in range(NCH):
                    for br in range(3):
                        pg, pgn = psr.next()
                        for k in range(NCH):
                            mm(P, pg[:, :T], pgn, wg[:, k, br * D + m * 128: br * D + (m + 1) * 128], h[:, k, :], ["wg", hn], k == 0, k == NCH - 1)
                        pb, pbn = psr.next()
                        y, yn = ys[br]
                        for k in range(NCH):
                            mm(P, pb[:, :T], pbn, wbr[br][:, k, m * 128:(m + 1) * 128], y[:, k, :], ["wbr%d" % br, yn], k == 0, k == NCH - 1)
                        sg, sgn = sgr.next()
                        P.op("act", lambda e, sg=sg, pg=pg: e.activation(out=sg[:], in_=pg[:, :T], func=AF.Sigmoid), reads=[pgn], writes=[sgn])
                        if br == 0:
                            P.op("dve", lambda e, sg=sg, pb=pb: e.tensor_tensor(out=macc[:], in0=pb[:, :T], in1=sg[:], op=ALU.mult),
                                 reads=[pbn, sgn], writes=["macc"])
                        else:
                            tt, ttn = tr.next()
                            P.op("dve", lambda e, sg=sg, pb=pb, tt=tt: e.tensor_tensor(out=tt[:], in0=pb[:, :T], in1=sg[:], op=ALU.mult),
                                 reads=[pbn, sgn], writes=[ttn])
                            if br == 1:
                                P.op("pool", lambda e, tt=tt: e.tensor_tensor(out=macc[:], in0=macc[:], in1=tt[:], op=ALU.add),
                                     reads=["macc", ttn], writes=["macc"])
                            else:
                                P.op("pool", lambda e, tt=tt, m=m: e.tensor_tensor(out=mg[:, m, :], in0=macc[:], in1=tt[:], op=ALU.add),
                                     reads=["macc", ttn], writes=["mg"])
                for m in range(NCH):
                    po, pon = psr.next()
                    for k in range(NCH):
                        mm(P, po[:, :T], pon, wo[:, k, m * 128:(m + 1) * 128], mg[:, k, :], ["wo", "mg"], k == 0, k == NCH - 1)
                    P.op("dve", lambda e, po=po, m=m, x=x: e.tensor_tensor(out=xm[:, m, :], in0=po[:, :T], in1=x[:, m, :], op=ALU.add),
                         reads=[pon, xn], writes=["xm"])
                norm_tile(P, T, xm, "xm", gf, ones, h2, "h2t", psr, (sq, "sq", rs, "rs"))
                P.dma("act", tview(xmid_d, t0, T), xm[:], reads=["xm"], writes=[("xmid", t0)])
                P.dma("act", tview(h2_d, t0, T), h2[:], reads=["h2t"], writes=[("h2", t0)])
            P.barrier()
        with contextlib.ExitStack() as st:
            P.stack = st
            wgu = P.sb("wgu", [128, NCH, 2 * FFN], BF16)
            wd = P.sb("wd", [128, NHC, D], BF16)
            load_w(P, wgu, wview(wgu_d), "wgu", 8)
            load_w(P, wd, wview(wd_d), "wd", 4)
            hr = Rot(P, "h2_", 2, [128, NCH, T], BF16)
            xr = Rot(P, "xm_", 2, [128, NCH, T])
            act = P.sb("act", [128, NHC, T], BF16)
            xnw = P.sb("xnw", [128, NCH, T])
            sq = P.sb("sq2", [128, NCH, T])
            rs = P.sb("rs2", [128, T])
            hn_t = P.sb("hnx", [128, NCH, T], F32 if last else BF16)
            sgr = Rot(P, "sl", 3, [128, T])
            for t0 in range(0, NT, T):
                h, hn = hr.next()
                P.dma("sp", h[:], tview(h2_d, t0, T), reads=[("h2", t0)], writes=[hn])
                x, xn = xr.next()
                P.dma("sp", x[:], tview(xmid_d, t0, T), reads=[("xmid", t0)], writes=[xn])
                for j in range(NHC):
                    pg, pgn = psr.next()
                    for k in range(NCH):
                        mm(P, pg[:, :T], pgn, wgu[:, k, j * 128:(j + 1) * 128], h[:, k, :], ["wgu", hn], k == 0, k == NCH - 1)
                    pu, pun = psr.next()
                    for k in range(NCH):
                        mm(P, pu[:, :T], pun, wgu[:, k, FFN + j * 128:FFN + (j + 1) * 128], h[:, k, :], ["wgu", hn], k == 0, k == NCH - 1)
                    sg, sgn = sgr.next()
                    P.op("act", lambda e, sg=sg, pg=pg: e.activation(out=sg[:], in_=pg[:, :T], func=AF.Silu), reads=[pgn], writes=[sgn])
                    P.op("dve", lambda e, sg=sg, pu=pu, j=j: e.tensor_tensor(out=act[:, j, :], in0=pu[:, :T], in1=sg[:], op=ALU.mult),
                         reads=[pun, sgn], writes=["actT"])
                for m in range(NCH):
                    pd, pdn = psr.next()
                    for j in range(NHC):
                        mm(P, pd[:, :T], pdn, wd[:, j, m * 128:(m + 1) * 128], act[:, j, :], ["wd", "actT"], j == 0, j == NHC - 1)
                    P.op("dve", lambda e, pd=pd, m=m, x=x: e.tensor_tensor(out=xnw[:, m, :], in0=pd[:, :T], in1=x[:, m, :], op=ALU.add),
                         reads=[pdn, xn], writes=["xnw"])
                norm_tile(P, T, xnw, "xnw", gn, ones, hn_t, "hnx", psr, (sq, "sq2", rs, "rs2"))
                P.dma("act", tview(xo, t0, T), xnw[:], reads=["xnw"])
                P.dma("act", tview(ho, t0, T), hn_t[:], reads=["hnx"])
            P.barrier()
        P.stack = st0
        P.finish()
    return nc


import os
DN_STAGE = int(os.environ.get('DN_STAGE', '3'))
DN_CUT = int(os.environ.get('DN_CUT', '99'))


class _Cut(Exception):
    pass


def cut(k):
    if DN_CUT == k:
        raise _Cut()


TA = 256
NFM = 18
NWA = NFM * 128 + 260
C_ID, C_U, C_SL, C_PERM, C_ONES = 0, 128, 256, 384, 512
V_CA, V_DC, V_DN, V_NEGA, V_DTB, NVEC = 0, 6, 30, 31, 33, 35


def host_consts():
    i = np.arange(128)
    cst = np.zeros((128, 640), np.float32)
    cst[:, C_ID:C_ID + 128] = np.eye(128)
    cst[:, C_U:C_U + 128] = (i[:, None] <= i[None, :])
    cst[:, C_SL:C_SL + 128] = (i[:, None] > i[None, :])
    cst[:, C_PERM:C_PERM + 128] = (i[:, None] == (i[None, :] + 64) % 128)
    cst[:, C_ONES:C_ONES + 128] = 1.0
    return cst


def V(P, eng, fn, reads, writes, **kw):
    P.op(eng, lambda e: getattr(e, fn)(**kw), reads=reads, writes=writes)


def build_a(S, do_conv=True, do_moba=True, do_dn=True):
    T = TA
    NB = S // T
    nc = bass.Bass("TRN2", target_bir_lowering=False)
    di = lambda n, s, dt=F32: nc.dram_tensor(n, list(s), dt, kind="ExternalInput").ap()
    hT = di("hT", [D, S], BF16)
    wA_d = di("wA", [D, NWA])
    cst_d = di("cst", [128, 640])
    vec_d = di("vec", [128, NVEC])
    cos_d = di("cosT", [128, S])
    sin_d = di("sinT", [128, S])
    yo = [nc.dram_tensor(n, [256, S], BF16, kind="ExternalOutput").ap() for n in ("ya", "yd", "ym")]
    SCALE = 128 ** -0.5
    with contextlib.ExitStack() as st:
        P = Prog(nc, st)
        cst = P.sb("cst", [128, 640])
        cstb = P.sb("cstb", [128, 640], BF16)
        vec = P.sb("vec", [128, NVEC])
        W = P.sb("W", [128, NCH, NWA], BF16)
        P.dma("sp", cst[:], cst_d, writes=["cst"])
        P.dma("sp", vec[:], vec_d, writes=["vec"])
        load_w(P, W, wview(wA_d), "W", 4)
        V(P, "pool", "tensor_copy", ["cst"], ["cstb"], out=cstb[:], in_=cst[:])
        ident = cst[:, C_ID:C_ID + 128]
        Um = cst[:, C_U:C_U + 128]
        SLm = cst[:, C_SL:C_SL + 128]
        ones = cst[:, C_ONES:C_ONES + 128]
        identb = cstb[:, C_ID:C_ID + 128]
        Ub = cstb[:, C_U:C_U + 128]
        permb = cstb[:, C_PERM:C_PERM + 128]
        CR = ["cst", "cstb", "vec"]

        psr = Rot(P, "ps", 6, [128, 512], F32, psum=True)
        ptr = Rot(P, "pt", 2, [128, 1024], BF16, psum=True)
        hr = Rot(P, "h", 2, [128, NCH, T], BF16)
        pj = [None if 6 <= i < 12 else P.sb("pj%d" % i, [128, T]) for i in range(NFM)]
        ext = {ci: P.sb("ext%d" % ci, [128, T + 3]) for ci in range(6, 12)}
        uext = [P.sb("uext%d" % i, [128, T + 2]) for i in range(2)]
        for ci in range(6, 12):
            V(P, "pool", "memset", [], ["ext%d" % ci], ap=ext[ci][:], constant=0.0)
        for i in range(2):
            V(P, "pool", "memset", [], ["uext%d" % i], ap=uext[i][:], constant=0.0)
        tmpr = Rot(P, "tmp", 4, [128, T])
        outr = Rot(P, "yout", 6, [128, T], BF16)
        KT = P.sb("KT", [128, 2, S], BF16)
        VA = P.sb("VA", [128, 2 * NB, 2, 130], BF16)
        kmT = P.sb("kmT", [128, 2, max(NB, 8)])
        V(P, "pool", "memset", [], ["VA"], ap=VA[:], constant=1.0)
        cosr = Rot(P, "cos", 2, [128, T])
        sinr = Rot(P, "sin", 2, [128, T])
        qr32 = [P.sb("qr32_%d" % h, [128, T]) for h in range(2)]
        qrb = [P.sb("qrb_%d" % h, [128, T], BF16) for h in range(2)]
        kr32 = P.sb("kr32", [128, T])
        rb = Rot(P, "rb", 2, [128, T], BF16)
        gbuf = Rot(P, "gbuf", 2, [128, 32])
        mx8 = Rot(P, "mx8", 2, [128, 8])
        selr = Rot(P, "sel", 4, [128, 32])
        pTr = Rot(P, "pT", 3, [128, 2, T], BF16)
        accr = Rot(P, "acc", 4, [128, 130])
        smallr = Rot(P, "sm", 8, [128, 4])
        obr = Rot(P, "ob", 2, [128, 128], BF16)
        Sst = [P.sb("S%d" % h, [128, 128]) for h in range(2)]
        Sbf = [P.sb("Sb%d" % h, [128, 128], BF16) for h in range(2)]
        for h in range(2):
            V(P, "pool", "memset", [], ["S%d" % h], ap=Sst[h][:], constant=0.0)
            V(P, "pool", "memset", [], ["Sb%d" % h], ap=Sbf[h][:], constant=0.0)
        cv = {ci: P.sb("cv%d" % ci, [128, T]) for ci in range(6, 12)}
        nrm = {ci: P.sb("nrm%d" % ci, [128, T]) for ci in range(6, 10)}
        nrmb = {ci: P.sb("nrmb%d" % ci, [128, T], BF16) for ci in range(6, 12)}
        tok = Rot(P, "tok", 2, [128, 16])
        f32r = Rot(P, "f32r", 4, [128, 128])
        scr = Rot(P, "scr", 2, [128, 24])
        negA = P.sb("negA", [128, 2])
        V(P, "act", "activation", ["vec"], ["negA"], out=negA[:], in_=vec[:, V_NEGA:V_NEGA + 2], func=AF.Exp)
        V(P, "dve", "tensor_scalar", ["negA"], ["negA"], out=negA[:], in0=negA[:], scalar1=-1.0, scalar2=None, op0=ALU.mult)
        ch = {}
        if do_dn:
            for hh in range(2):
                for cc in range(2):
                    n = "c%d%d_" % (hh, cc)
                    X = {"n": n}
                    for nm in ("R", "Gm", "Ds", "DTc", "XT"):
                        X[nm] = P.sb(n + nm, [128, 128])
                    X["EX"] = P.sb(n + "EX", [128, 384])
                    for nm in ("XTb", "QK", "kg", "qg", "kd", "vb", "r", "vn", "on"):
                        X[nm] = P.sb(n + nm, [128, 128], BF16)
                    X["PP"] = [P.sb(n + "PP%d" % i, [128, 256], BF16) for i in range(2)]
                    X["P"] = [t[:, 0:128] for t in X["PP"]]
                    X["PT"] = [t[:, 128:256] for t in X["PP"]]
                    ch[(hh, cc)] = X

        for b in range(NB):
            t0 = b * T
            h_t, hn = hr.next()
            P.dma("sp", h_t[:], tview(hT, t0, T), writes=[hn])
            cs, csn = cosr.next()
            sn, snn = sinr.next()
            P.dma("sp", cs[:], cos_d[:, t0:t0 + T], writes=[csn])
            P.dma("sp", sn[:], sin_d[:, t0:t0 + T], writes=[snn])
            if b > 0:
                for ci in range(6, 12):
                    V(P, "pool", "tensor_copy", ["ext%d" % ci], ["ext%d" % ci], out=ext[ci][:, 0:3], in_=ext[ci][:, T:T + 3])
                for i in range(2):
                    V(P, "pool", "tensor_copy", ["uext%d" % i], ["uext%d" % i], out=uext[i][:, 0:2], in_=uext[i][:, T:T + 2])
            for ci in range(NFM):
                ps, psn = psr.next()
                for k in range(NCH):
                    mm(P, ps[:, :T], psn, W[:, k, ci * 128:(ci + 1) * 128], h_t[:, k, :], ["W", hn], k == 0, k == NCH - 1)
                if ci in ext:
                    V(P, "act", "activation", [psn], ["ext%d" % ci], out=ext[ci][:, 3:T + 3], in_=ps[:, :T], func=AF.Copy)
                else:
                    V(P, "act", "activation", [psn], ["pj%d" % ci], out=pj[ci][:], in_=ps[:, :T], func=AF.Copy)
            tk, tkn = tok.next()
            for s in range(2):
                ps, psn = psr.next()
                for k in range(NCH):
                    mm(P, ps[:, :260], psn, h_t[:, k, s * 128:(s + 1) * 128], W[:, k, NFM * 128:NWA], ["W", hn], k == 0, k == NCH - 1)
                V(P, "act", "activation", [psn], ["VA"], out=VA[:, 2 * b + s, :, 0:128],
                  in_=ps[:, 0:256].rearrange("p (h d) -> p h d", h=2), func=AF.Copy)
                V(P, "dve", "tensor_copy", [psn], [tkn], out=tk[:, 4 * s:4 * s + 4], in_=ps[:, 256:260])

            if do_conv:
                for i in range(2):
                    ax, ac, ab = pj[i], pj[2 + i], pj[4 + i]
                    un = "uext%d" % i
                    V(P, "pool", "tensor_tensor", ["pj%d" % i, "pj%d" % (2 + i)], [un], out=uext[i][:, 2:T + 2], in0=ax[:], in1=ac[:], op=ALU.mult)
                    t1, t1n = tmpr.next()
                    V(P, "dve", "tensor_scalar", [un] + CR, [t1n], out=t1[:], in0=uext[i][:, 0:T], scalar1=vec[:, V_CA + 3 * i:V_CA + 3 * i + 1],
                      scalar2=None, op0=ALU.mult)
                    for j in (1, 2):
                        V(P, "dve", "scalar_tensor_tensor", [un, t1n] + CR, [t1n], out=t1[:], in0=uext[i][:, j:T + j],
                          scalar=vec[:, V_CA + 3 * i + j:V_CA + 3 * i + j + 1], in1=t1[:], op0=ALU.mult, op1=ALU.add)
                    yt, ytn = outr.next()
                    V(P, "pool", "tensor_tensor", [t1n, "pj%d" % (4 + i)], [ytn], out=yt[:], in0=t1[:], in1=ab[:], op=ALU.mult)
                    P.dma("sp", yo[0][i * 128:(i + 1) * 128, t0:t0 + T], yt[:], reads=[ytn])

            if do_moba:
                for h in range(2):
                    for which in ("k", "q"):
                        src = pj[16 + h] if which == "k" else pj[14 + h]
                        srcn = "pj%d" % ((16 if which == "k" else 14) + h)
                        xb, xbn = rb.next()
                        V(P, "pool", "tensor_copy", [srcn], [xbn], out=xb[:], in_=src[:])
                        ps, psn = psr.next()
                        mm(P, ps[:, :T], psn, permb, xb[:], [xbn, "cstb"], True, True)
                        t1, t1n = tmpr.next()
                        t2, t2n = tmpr.next()
                        V(P, "pool", "tensor_tensor", [srcn, csn], [t1n], out=t1[:], in0=src[:], in1=cs[:], op=ALU.mult)
                        V(P, "dve", "tensor_tensor", [psn, snn], [t2n], out=t2[:], in0=ps[:, :T], in1=sn[:], op=ALU.mult)
                        if which == "k":
                            V(P, "pool", "tensor_tensor", [t1n, t2n], ["kr32"], out=kr32[:], in0=t1[:], in1=t2[:], op=ALU.add)
                            V(P, "pool", "tensor_copy", ["kr32"], ["KT"], out=KT[:, h, t0:t0 + T], in_=kr32[:])
                            sm, smn = smallr.next()
                            V(P, "dve", "tensor_reduce", ["kr32"], [smn], out=sm[:, 0:1], in_=kr32[:], axis=AX.X, op=ALU.add)
                            V(P, "dve", "tensor_scalar", [smn], ["kmT"], out=kmT[:, h, b:b + 1], in0=sm[:, 0:1], scalar1=1.0 / T, scalar2=None,
                              op0=ALU.mult)
                        else:
                            V(P, "pool", "tensor_tensor", [t1n, t2n], ["qr32_%d" % h], out=qr32[h][:], in0=t1[:], in1=t2[:], op=ALU.add)
                            V(P, "pool", "tensor_copy", ["qr32_%d" % h], ["qrb_%d" % h], out=qrb[h][:], in_=qr32[h][:])
                    qn32, qnb = "qr32_%d" % h, "qrb_%d" % h
                    sels = []
                    if b > 0:
                        for qs in range(2):
                            ps, psn = psr.next()
                            mm(P, ps[:, :b], psn, qr32[h][:, qs * 128:(qs + 1) * 128], kmT[:, h, 0:b], [qn32, "kmT"], True, True)
                            gb, gbn = gbuf.next()
                            V(P, "pool", "memset", [], [gbn], ap=gb[:], constant=-1e30)
                            V(P, "dve", "tensor_copy", [psn], [gbn], out=gb[:, 0:b], in_=ps[:, :b])
                            m8, m8n = mx8.next()
                            V(P, "dve", "max", [gbn], [m8n], out=m8[:], in_=gb[:])
                            sl, sln = selr.next()
                            V(P, "dve", "tensor_scalar", [gbn, m8n], [sln], out=sl[:], in0=gb[:], scalar1=m8[:, 2:3], scalar2=None, op0=ALU.is_ge)
                            sels.append((sl, sln))
                    accs = []
                    ps, psn = psr.next()
                    for hf in range(2):
                        mm(P, ps[:, hf * T:(hf + 1) * T], psn, KT[:, h, t0 + hf * 128:t0 + (hf + 1) * 128], qrb[h][:], ["KT", qnb], True, True)
                    pT, pTn = pTr.next()
                    V(P, "act", "activation", [psn], [pTn], out=pT[:], in_=ps[:].rearrange("p (a q) -> p a q", a=2), func=AF.Exp, scale=SCALE)
                    V(P, "pool", "tensor_tensor", [pTn, "cstb"], [pTn], out=pT[:, 0, 0:128], in0=pT[:, 0, 0:128], in1=Ub, op=ALU.mult)
                    V(P, "pool", "tensor_tensor", [pTn, "cstb"], [pTn], out=pT[:, 1, 128:256], in0=pT[:, 1, 128:256], in1=Ub, op=ALU.mult)
                    po, pon = psr.next()
                    mm(P, po[:, 0:129], pon, pT[:, 0, 0:128], VA[:, 2 * b, h, 0:129], [pTn, "VA"], True, True)
                    mm(P, po[:, 256:385], pon, pT[:, 0, 128:256], VA[:, 2 * b, h, 0:129], [pTn, "VA"], True, False)
                    mm(P, po[:, 256:385], pon, pT[:, 1, 128:256], VA[:, 2 * b + 1, h, 0:129], [pTn, "VA"], False, True)
                    for qs in range(2):
                        ac, acn = accr.next()
                        V(P, "act", "activation", [pon], [acn], out=ac[:, 0:129], in_=po[:, qs * 256:qs * 256 + 129], func=AF.Copy)
                        accs.append((ac, acn))
                    for j in range(b):
                        ps, psn = psr.next()
                        for hf in range(2):
                            mm(P, ps[:, hf * T:(hf + 1) * T], psn, KT[:, h, j * T + hf * 128:j * T + (hf + 1) * 128], qrb[h][:], ["KT", qnb], True, True)
                        pT, pTn = pTr.next()
                        V(P, "act", "activation", [psn], [pTn], out=pT[:], in_=ps[:].rearrange("p (a q) -> p a q", a=2), func=AF.Exp, scale=SCALE)
                        po, pon = psr.next()
                        for qs in range(2):
                            for hf in range(2):
                                mm(P, po[:, qs * 256:qs * 256 + 129], pon, pT[:, hf, qs * 128:(qs + 1) * 128], VA[:, 2 * j + hf, h, 0:129],
                                   [pTn, "VA"], hf == 0, hf == 1)
                        for qs in range(2):
                            ac, acn = accs[qs]
                            sl, sln = sels[qs]
                            V(P, "dve", "scalar_tensor_tensor", [pon, sln, acn], [acn], out=ac[:, 0:129], in0=po[:, qs * 256:qs * 256 + 129],
                              scalar=sl[:, j:j + 1], in1=ac[:, 0:129], op0=ALU.mult, op1=ALU.add)
                    yt, ytn = outr.next()
                    for qs in range(2):
                        ac, acn = accs[qs]
                        sm, smn = smallr.next()
                        V(P, "dve", "reciprocal", [acn], [smn], out=sm[:, 0:1], in_=ac[:, 128:129])
                        ob, obn = obr.next()
                        V(P, "act", "activation", [acn, smn], [obn], out=ob[:], in_=ac[:, 0:128], func=AF.Copy, scale=sm[:, 0:1])
                        pt, ptn = ptr.next()
                        V(P, "pe", "transpose", [obn, "cstb"], [ptn], out=pt[:, 0:128], in_=ob[:], identity=identb)
                        V(P, "dve", "tensor_copy", [ptn], [ytn], out=yt[:, qs * 128:(qs + 1) * 128], in_=pt[:, 0:128])
                    P.dma("sp", yo[2][h * 128:(h + 1) * 128, t0:t0 + T], yt[:], reads=[ytn])

            if do_dn:

                try:
                    for h in range(2):
                        for ci in (6 + h, 8 + h, 10 + h):
                            en = "ext%d" % ci
                            wc = V_DC + 4 * (ci - 6)
                            t1, t1n = tmpr.next()
                            V(P, "dve", "tensor_scalar", [en] + CR, [t1n], out=t1[:], in0=ext[ci][:, 0:T], scalar1=vec[:, wc:wc + 1], scalar2=None, op0=ALU.mult)
                            for j in (1, 2, 3):
                                V(P, "dve", "scalar_tensor_tensor", [en, t1n] + CR, [t1n], out=t1[:], in0=ext[ci][:, j:T + j],
                                  scalar=vec[:, wc + j:wc + j + 1], in1=t1[:], op0=ALU.mult, op1=ALU.add)
                            V(P, "act", "activation", [t1n], ["cv%d" % ci], out=cv[ci][:], in_=t1[:], func=AF.Silu)
                        for ci in (6 + h, 8 + h):
                            t1, t1n = tmpr.next()
                            V(P, "pool", "tensor_tensor", ["cv%d" % ci], [t1n], out=t1[:], in0=cv[ci][:], in1=cv[ci][:], op=ALU.mult)
                            ps, psn = psr.next()
                            mm(P, ps[:, :T], psn, ones, t1[:], [t1n, "cst"], True, True)
                            t2, t2n = tmpr.next()
                            V(P, "dve", "tensor_scalar", [psn], [t2n], out=t2[:], in0=ps[:, :T], scalar1=EPS, scalar2=None, op0=ALU.add)
                            V(P, "act", "activation", [t2n], [t2n], out=t2[:], in_=t2[:], func=AF.Sqrt)
                            V(P, "dve", "reciprocal", [t2n], [t2n], out=t2[:], in_=t2[:])
                            V(P, "dve", "scalar_tensor_tensor", ["cv%d" % ci, t2n], ["nrm%d" % ci], out=nrm[ci][:], in0=cv[ci][:],
                              scalar=(SCALE if ci < 8 else 1.0), in1=t2[:], op0=ALU.mult, op1=ALU.mult)
                            V(P, "pool", "tensor_copy", ["nrm%d" % ci], ["nrmb%d" % ci], out=nrmb[ci][:], in_=nrm[ci][:])
                        V(P, "pool", "tensor_copy", ["cv%d" % (10 + h)], ["nrmb%d" % (10 + h)], out=nrmb[10 + h][:], in_=cv[10 + h][:])
                        V(P, "act", "activation", ["pj%d" % (12 + h)], ["pj%d" % (12 + h)], out=pj[12 + h][:], in_=pj[12 + h][:], func=AF.Silu)
                    cut(1)
                    tk3 = tk[:, 0:8].rearrange("p (s f) -> p s f", s=2)
                    braw, araw = tk3[:, :, 0:2], tk3[:, :, 2:4]
                    sc, scn = scr.next()
                    sc4 = sc[:].rearrange("p (k s h) -> p k s h", k=6, s=2)
                    V(P, "act", "activation", [tkn], [scn], out=sc4[:, 0], in_=braw, func=AF.Sigmoid)
                    V(P, "dve", "tensor_scalar", [scn], [scn], out=sc4[:, 1], in0=sc4[:, 0], scalar1=-1.0, scalar2=None, op0=ALU.mult)
                    dtb_b = vec[:, V_DTB:V_DTB + 2].rearrange("p (o h) -> p o h", o=1).broadcast_to([128, 2, 2])
                    nega_b = negA[:, 0:2].rearrange("p (o h) -> p o h", o=1).broadcast_to([128, 2, 2])
                    V(P, "dve", "tensor_tensor", [tkn] + CR, [scn], out=sc4[:, 3], in0=araw, in1=dtb_b, op=ALU.add)
                    V(P, "dve", "tensor_scalar", [scn], [scn], out=sc4[:, 4], in0=sc4[:, 3], scalar1=-1.0, scalar2=None, op0=ALU.mult)
                    V(P, "dve", "tensor_tensor", [scn], [scn], out=sc4[:, 4], in0=sc4[:, 4], in1=sc4[:, 3], op=ALU.max)
                    V(P, "act", "activation", [scn], [scn], out=sc4[:, 4], in_=sc4[:, 4], func=AF.Exp, scale=-1.0)
                    V(P, "dve", "tensor_scalar", [scn], [scn], out=sc4[:, 4], in0=sc4[:, 4], scalar1=1.0, scalar2=None, op0=ALU.add)
                    V(P, "act", "activation", [scn], [scn], out=sc4[:, 4], in_=sc4[:, 4], func=AF.Ln)
                    V(P, "dve", "tensor_scalar", [scn], [scn], out=sc4[:, 5], in0=sc4[:, 3], scalar1=0.0, scalar2=None, op0=ALU.max)
                    V(P, "dve", "tensor_tensor", [scn], [scn], out=sc4[:, 5], in0=sc4[:, 5], in1=sc4[:, 4], op=ALU.add)
                    V(P, "dve", "tensor_tensor", [scn, "negA"], [scn], out=sc4[:, 2], in0=sc4[:, 5], in1=nega_b, op=ALU.mult)
                    cut(2)
                    chains = [(h, c) for c in range(2) for h in range(2)]
                    cols = lambda c: slice(c * 128, (c + 1) * 128)

                    def col(kk, c, h):
                        return sc4[:, kk, c, h:h + 1]

                    for (h, c) in chains:
                        X = ch[(h, c)]
                        V(P, "dve", "tensor_scalar", [scn] + CR, [X["n"] + "R"], out=X["R"][:], in0=SLm, scalar1=col(2, c, h), scalar2=None, op0=ALU.mult)
                        V(P, "dve", "tensor_scalar", [scn] + CR, [X["n"] + "Gm"], out=X["Gm"][:], in0=ones, scalar1=col(2, c, h), scalar2=None, op0=ALU.mult)
                    cut(3)
                    for (h, c) in chains:
                        X = ch[(h, c)]
                        ps, psn = psr.next()
                        mm(P, ps[:, 0:128], psn, Um, X["R"][:], [X["n"] + "R", "cst"], True, True)
                        mm(P, ps[:, 128:256], psn, X["R"][:], Um, [X["n"] + "R", "cst"], True, True)
                        mm(P, ps[:, 256:384], psn, X["Gm"][:], Um, [X["n"] + "Gm", "cst"], True, True)
                        V(P, "act", "activation", [psn], [X["n"] + "EX"], out=X["EX"][:], in_=ps[:, 0:384], func=AF.Exp)
                        V(P, "pool", "tensor_tensor", [X["n"] + "EX", "cst"], [X["n"] + "Ds"], out=X["Ds"][:], in0=X["EX"][:, 0:128], in1=SLm, op=ALU.mult)
                        V(P, "pool", "tensor_tensor", [X["n"] + "EX", "cst"], [X["n"] + "DTc"], out=X["DTc"][:], in0=X["EX"][:, 128:256], in1=Um, op=ALU.mult)
                    cut(4)
                    for (h, c) in chains:
                        X = ch[(h, c)]
                        kb, qb = nrmb[8 + h], nrmb[6 + h]
                        ps, psn = psr.next()
                        mm(P, ps[:, 0:128], psn, kb[:, cols(c)], kb[:, cols(c)], ["nrmb%d" % (8 + h)], True, True)
                        mm(P, ps[:, 128:256], psn, kb[:, cols(c)], qb[:, cols(c)], ["nrmb%d" % (8 + h), "nrmb%d" % (6 + h)], True, True)
                        V(P, "dve", "scalar_tensor_tensor", [psn, scn, X["n"] + "Ds"], [X["n"] + "P0"], out=X["P"][0], in0=ps[:, 0:128],
                          scalar=col(0, c, h), in1=X["Ds"][:], op0=ALU.mult, op1=ALU.mult)
                        V(P, "dve", "tensor_tensor", [psn, X["n"] + "DTc"], [X["n"] + "QK"], out=X["QK"][:], in0=ps[:, 128:256], in1=X["DTc"][:], op=ALU.mult)
                        V(P, "pool", "tensor_tensor", ["nrm%d" % (8 + h), X["n"] + "EX"], [X["n"] + "kg"], out=X["kg"][:], in0=nrm[8 + h][:, cols(c)],
                          in1=X["EX"][:, 256:384], op=ALU.mult)
                        V(P, "pool", "tensor_tensor", ["nrm%d" % (6 + h), X["n"] + "EX"], [X["n"] + "qg"], out=X["qg"][:], in0=nrm[6 + h][:, cols(c)],
                          in1=X["EX"][:, 256:384], op=ALU.mult)
                    cut(5)
                    for (h, c) in chains:
                        X = ch[(h, c)]
                        pt, ptn = ptr.next()
                        V(P, "pe", "transpose", [X["n"] + "P0", "cstb"], [ptn], out=pt[:, 0:128], in_=X["P"][0], identity=identb)
                        V(P, "pe", "transpose", ["nrmb%d" % (8 + h), "cstb"], [ptn], out=pt[:, 128:256], in_=nrmb[8 + h][:, cols(c)], identity=identb)
                        V(P, "pe", "transpose", ["nrmb%d" % (10 + h), "cstb"], [ptn], out=pt[:, 256:384], in_=nrmb[10 + h][:, cols(c)], identity=identb)
                        V(P, "dve", "tensor_copy", [ptn], [X["n"] + "PT0"], out=X["PT"][0], in_=pt[:, 0:128])
                        V(P, "pool", "tensor_tensor", [X["n"] + "PT0", "cst"], [X["n"] + "XT"], out=X["XT"][:], in0=ident, in1=X["PT"][0], op=ALU.subtract)
                        V(P, "pool", "tensor_copy", [X["n"] + "XT"], [X["n"] + "XTb"], out=X["XTb"][:], in_=X["XT"][:])
                        V(P, "dve", "tensor_copy", [ptn], [X["n"] + "kd"], out=X["kd"][:], in_=pt[:, 128:256])
                        V(P, "dve", "tensor_scalar", [X["n"] + "kd", X["n"] + "EX"], [X["n"] + "kd"], out=X["kd"][:], in0=X["kd"][:],
                          scalar1=X["EX"][:, 255:256], scalar2=None, op0=ALU.mult)
                        V(P, "dve", "tensor_copy", [ptn], [X["n"] + "vb"], out=X["vb"][:], in_=pt[:, 256:384])
                        V(P, "dve", "tensor_scalar", [X["n"] + "vb", scn], [X["n"] + "vb"], out=X["vb"][:], in0=X["vb"][:],
                          scalar1=col(0, c, h), scalar2=None, op0=ALU.mult)
                    cut(6)
                    for m in range(1, 1 + int(os.environ.get('DN_LV', '6'))):
                        a, bb = (m - 1) % 2, m % 2
                        for (h, c) in chains:
                            X = ch[(h, c)]
                            n = X["n"]
                            ps, psn = psr.next()
                            mm(P, ps[:, 0:128], psn, X["PT"][a], X["P"][a], [n + "P%d" % a, n + "PT%d" % a], True, True)
                            if m < 6:
                                mm(P, ps[:, 128:256], psn, X["P"][a], X["PT"][a], [n + "P%d" % a, n + "PT%d" % a], True, True)
                                V(P, "dve", "tensor_copy", [psn], [n + "P%d" % bb, n + "PT%d" % bb], out=X["PP"][bb][:], in_=ps[:, 0:256])
                            else:
                                V(P, "dve", "tensor_copy", [psn], [n + "P%d" % bb], out=X["P"][bb], in_=ps[:, 0:128])
                        for (h, c) in (chains if os.environ.get('DN_NOX', '0') == '0' else []):
                            X = ch[(h, c)]
                            n = X["n"]
                            ps, psn = psr.next()
                            mm(P, ps[:, 0:128], psn, X["P"][bb], X["XTb"][:], [n + "P%d" % bb, n + "XTb"], True, True)
                            V(P, "dve", "tensor_tensor", [psn, n + "XT"], [n + "XT"], out=X["XT"][:], in0=ps[:, 0:128], in1=X["XT"][:], op=ALU.add)
                            V(P, "pool", "tensor_copy", [n + "XT"], [n + "XTb"], out=X["XTb"][:], in_=X["XT"][:])
                    yts = [outr.next() for _ in range(2)]
                    for c in range(2 if DN_STAGE >= 2 else 0):
                        pss = {}
                        for h in range(2):
                            X = ch[(h, c)]
                            n = X["n"]
                            ps, psn = psr.next()
                            pss[h] = (ps, psn)
                            mm(P, ps[:, 0:128], psn, X["kg"][:], Sbf[h][:], [n + "kg", "Sb%d" % h], True, True)
                            V(P, "dve", "scalar_tensor_tensor", [psn, scn, n + "vb"], [n + "r"], out=X["r"][:], in0=ps[:, 0:128], scalar=col(1, c, h),
                              in1=X["vb"][:], op0=ALU.mult, op1=ALU.add)
                        for h in range(2):
                            X = ch[(h, c)]
                            n = X["n"]
                            ps, psn = pss[h]
                            mm(P, ps[:, 128:256], psn, X["XTb"][:], X["r"][:], [n + "XTb", n + "r"], True, True)
                            V(P, "act", "activation", [psn], [n + "vn"], out=X["vn"][:], in_=ps[:, 128:256], func=AF.Copy)
                        for h in range(2):
                            X = ch[(h, c)]
                            n = X["n"]
                            ps, psn = pss[h]
                            po, pon = psr.next()
                            mm(P, po[:, 0:128], pon, X["qg"][:], Sbf[h][:], [n + "qg", "Sb%d" % h], True, False)
                            mm(P, po[:, 0:128], pon, X["QK"][:], X["vn"][:], [n + "QK", n + "vn"], False, True)
                            mm(P, ps[:, 256:384], psn, X["kd"][:], X["vn"][:], [n + "kd", n + "vn"], True, True)
                            V(P, "dve", "scalar_tensor_tensor", [psn, n + "EX", "S%d" % h], ["S%d" % h], out=Sst[h][:], in0=Sst[h][:],
                              scalar=X["EX"][:, 383:384], in1=ps[:, 256:384], op0=ALU.mult, op1=ALU.add)
                            V(P, "pool", "tensor_copy", ["S%d" % h], ["Sb%d" % h], out=Sbf[h][:], in_=Sst[h][:])
                            if DN_STAGE < 3:
                                continue
                            sm, smn = smallr.next()
                            t1, t1n = f32r.next()
                            V(P, "act", "activation", [pon], [t1n, smn], out=t1[:], in_=po[:, 0:128], func=AF.Square, accum_out=sm[:, 0:1])
                            V(P, "dve", "tensor_scalar", [smn], [smn], out=sm[:, 1:2], in0=sm[:, 0:1], scalar1=1.0 / 128, scalar2=EPS, op0=ALU.mult, op1=ALU.add)
                            V(P, "act", "activation", [smn], [smn], out=sm[:, 2:3], in_=sm[:, 1:2], func=AF.Sqrt)
                            V(P, "dve", "reciprocal", [smn], [smn], out=sm[:, 3:4], in_=sm[:, 2:3])
                            V(P, "act", "activation", [pon, smn], [n + "on"], out=X["on"][:], in_=po[:, 0:128], func=AF.Copy, scale=sm[:, 3:4])
                            pt, ptn = ptr.next()
                            V(P, "pe", "transpose", [n + "on", "cstb"], [ptn], out=pt[:, 0:128], in_=X["on"][:], identity=identb)
                            yt, ytn = yts[h]
                            V(P, "dve", "tensor_copy", [ptn], [n + "on"], out=X["on"][:], in_=pt[:, 0:128])
                            V(P, "dve", "scalar_tensor_tensor", [n + "on", "pj%d" % (12 + h)] + CR, [ytn], out=yt[:, cols(c)], in0=X["on"][:],
                              scalar=vec[:, V_DN:V_DN + 1], in1=pj[12 + h][:, cols(c)], op0=ALU.mult, op1=ALU.mult)
                    for h in range(2 if DN_STAGE >= 3 else 0):
                        yt, ytn = yts[h]
                        P.dma("sp", yo[1][h * 128:(h + 1) * 128, t0:t0 + T], yt[:], reads=[ytn])
                except _Cut:
                    pass
        P.finish()
    return nc


OFF_AX, OFF_AC, OFF_AB, OFF_DQ, OFF_DK, OFF_DV, OFF_DZ, OFF_DB, OFF_DA, OFF_MQ, OFF_MK, OFF_MV, OFF_G = (
    0, 1024, 2048, 3072, 4096, 5120, 6144, 7168, 7176, 7184, 8208, 9232, 10256)


def lay8(g):
    return np.ascontiguousarray(np.asarray(g, np.float32).reshape(NCH, 128).T)


def rope_tables_T(S):
    inv = (1.0 / (np.float32(10000.0) ** (np.arange(0, 128, 2, dtype=np.float32) / np.float32(128)))).astype(np.float32)
    ang = (np.arange(S, dtype=np.float32)[:, None] * inv[None, :]).astype(np.float32)
    c = np.cos(ang.astype(np.float64)).astype(np.float32).T
    s = np.sin(ang.astype(np.float64)).astype(np.float32).T
    cosT = np.ascontiguousarray(np.concatenate([c, c], 0))
    sinT = np.ascontiguousarray(np.concatenate([-s, s], 0))
    return cosT, sinT


def prep_a(w_in_l, conv_a_w_l, dn_conv_w_l, a_log_l, dt_bias_l, dn_norm_l, r):
    c0 = 256 * r
    cols = []
    for off in (OFF_AX, OFF_AC, OFF_AB, OFF_DQ, OFF_DK, OFF_DV, OFF_DZ, OFF_MQ, OFF_MK, OFF_MV):
        cols.append(np.arange(off + c0, off + c0 + 256))
    cols.append(np.arange(OFF_DB + 2 * r, OFF_DB + 2 * r + 2))
    cols.append(np.arange(OFF_DA + 2 * r, OFF_DA + 2 * r + 2))
    cols = np.concatenate(cols)
    wA = np.ascontiguousarray(w_in_l[:, cols])
    vec = np.zeros((128, NVEC), np.float32)
    for i in range(2):
        for j in range(3):
            vec[:, V_CA + 3 * i + j] = conv_a_w_l[j, c0 + i * 128:c0 + (i + 1) * 128]
    for ci in range(6):
        which, hh = ci // 2, ci % 2
        for j in range(4):
            vec[:, V_DC + 4 * ci + j] = dn_conv_w_l[j, which * 1024 + c0 + hh * 128: which * 1024 + c0 + (hh + 1) * 128]
    vec[:, V_DN] = dn_norm_l
    for h in range(2):
        vec[:, V_NEGA + h] = a_log_l[2 * r + h]
        vec[:, V_DTB + h] = dt_bias_l[2 * r + h]
    return wA, vec


_NC = {}


def _get(name, fn):
    if name not in _NC:
        _NC[name] = fn()
    return _NC[name]


def kernel(x, attn_norm, w_in, conv_a_w, dn_conv_w, dn_a_log, dn_dt_bias, dn_norm,
           w_br_a, w_br_dn, w_br_moba, w_out, ffn_norm, w_gate_up, w_down, final_norm):
    f = lambda a: np.ascontiguousarray(np.asarray(a, dtype=np.float32))
    x = f(x)
    Bn, S, _ = x.shape
    NT = S // 4
    cores = list(range(8))
    ones = np.ones((128, 128), np.float32)
    cst = host_consts()
    cosT, sinT = rope_tables_T(S)
    xT = [np.ascontiguousarray(x[c // 4, (c % 4) * NT:(c % 4 + 1) * NT, :].T) for c in cores]
    nc0 = _get("p0", lambda: build_p0(NT))
    g0 = lay8(attn_norm[0])
    res = run_bass_kernel_spmd(nc0, [{"xT": xT[c], "gam": g0, "ones": ones} for c in cores], core_ids=cores)
    hT = [np.asarray(res.results[c]["hT"]) for c in cores]
    out = None
    for l in range(DEPTH):
        last = l == DEPTH - 1
        hfull = [np.ascontiguousarray(np.concatenate([hT[4 * b + r] for r in range(4)], axis=1)) for b in range(Bn)]
        nca = _get("a", lambda: build_a(S))
        preps = [prep_a(f(w_in[l]), f(conv_a_w[l]), f(dn_conv_w[l]), f(dn_a_log[l]), f(dn_dt_bias[l]), f(dn_norm[l]), r) for r in range(4)]
        ins = [{"hT": hfull[c // 4], "wA": preps[c % 4][0], "vec": preps[c % 4][1], "cst": cst, "cosT": cosT, "sinT": sinT} for c in cores]
        res = run_bass_kernel_spmd(nca, ins, core_ids=cores)
        ys = []
        for nm in ("ya", "yd", "ym"):
            ys.append([np.concatenate([np.asarray(res.results[4 * b + r][nm]) for r in range(4)], axis=0) for b in range(Bn)])
        ncb = _get("bl" if last else "b", lambda: build_b(NT, last))
        wl = f(w_in[l])
        common = {"wg": np.ascontiguousarray(wl[:, OFF_G:]), "wbr0": f(w_br_a[l]), "wbr1": f(w_br_dn[l]), "wbr2": f(w_br_moba[l]),
                  "wo": f(w_out[l]), "wgu": f(w_gate_up[l]), "wd": f(w_down[l]), "gffn": lay8(ffn_norm[l]),
                  "gnext": lay8(final_norm if last else attn_norm[l + 1]), "ones": ones}
        ins = []
        for c in cores:
            b, r = c // 4, c % 4
            d = dict(common)
            d["xT"] = xT[c]
            d["hT"] = hT[c]
            for i in range(3):
                d["y%d" % i] = np.ascontiguousarray(ys[i][b][:, r * NT:(r + 1) * NT])
            ins.append(d)
        res = run_bass_kernel_spmd(ncb, ins, core_ids=cores)
        xT = [np.asarray(res.results[c]["xo"]) for c in cores]
        if last:
            out = np.zeros((Bn, S, D), np.float32)
            for c in cores:
                out[c // 4, (c % 4) * NT:(c % 4 + 1) * NT, :] = np.asarray(res.results[c]["of"]).T
        else:
            hT = [np.asarray(res.results[c]["ho"]) for c in cores]
    return out
```
